# Optimizing a Trainium2 kernel written in Bass

```python
import jax, jax.numpy as jnp
from jax import lax
import numpy as np

D_MODEL = 1024
BATCH = 1
SEQ = 16384
DEPTH = 2

N_EVEN = (DEPTH + 1) // 2
N_ODD = DEPTH // 2
EPS = 1e-6

CHUNK = 128
A_WIDTH = D_MODEL // 2
A_GROUPS = 4
A_HD = A_WIDTH // A_GROUPS
B_WIDTH = D_MODEL // 2
POOL_WINDOWS = (2, 4, 8, 16)
B_GROUPS = len(POOL_WINDOWS)
B_HD = B_WIDTH // B_GROUPS
E_IN = 2 * A_WIDTH + B_WIDTH
E_OUT = A_WIDTH + B_WIDTH
C_WIDTH = D_MODEL // 2
CONV_W = 3
MLA_HEADS = 4
Q_LORA = 256
KV_LORA = 256
QK_NOPE = 128
QK_ROPE = 64
QK_HD = QK_NOPE + QK_ROPE
V_HD = 128
ROPE_THETA = 10000.0
Q_BLOCK = 128
O_IN = 3 * C_WIDTH + Q_LORA + KV_LORA + QK_ROPE
O_OUT = C_WIDTH + MLA_HEADS * V_HD
D_FF = 2816
N_EXPERTS = 8
TOP_K = 2
D_FF_EXPERT = 3584

kernel_name = "hybrid_gmlp_pool_conv_mla_moe_adaln"


def rmsnorm(x, w):
    xf = x.astype(jnp.float32)
    y = xf * lax.rsqrt(jnp.mean(xf * xf, axis=-1, keepdims=True) + EPS)
    return (y * w.astype(jnp.float32)).astype(x.dtype)


def layernorm(x, w, b):
    xf = x.astype(jnp.float32)
    mu = jnp.mean(xf, axis=-1, keepdims=True)
    var = jnp.mean(jnp.square(xf - mu), axis=-1, keepdims=True)
    y = (xf - mu) * lax.rsqrt(var + EPS)
    return (y * w.astype(jnp.float32) + b.astype(jnp.float32)).astype(x.dtype)


def swiglu(h, w_gu, w_down):
    g, u = jnp.split(h @ w_gu, 2, axis=-1)
    return (jax.nn.silu(g) * u) @ w_down


def mixer_a(uv, ln_w, ln_b, w_s, b_s):
    bsz, s, _ = uv.shape
    u, v = jnp.split(jax.nn.gelu(uv), 2, axis=-1)
    v = layernorm(v, ln_w, ln_b)
    v = v.reshape(bsz, s // CHUNK, CHUNK, A_GROUPS, A_HD)
    w = w_s * jnp.tril(jnp.ones((CHUNK, CHUNK), dtype=w_s.dtype))[None]
    sv = jnp.einsum('gts,bnsgd->bntgd', w, v) + b_s.T[None, None, :, :, None]
    return u * sv.reshape(bsz, s, A_WIDTH)


def mixer_b(p, w_grp, scale):
    bsz, s, _ = p.shape
    cs = jnp.pad(jnp.cumsum(p.astype(jnp.float32), axis=1), ((0, 0), (1, 0), (0, 0)))
    t = jnp.arange(s)
    pooled = []
    for gi, win in enumerate(POOL_WINDOWS):
        csg = cs[..., gi * B_HD:(gi + 1) * B_HD]
        hi = csg[:, 1:]
        lo = jnp.pad(csg, ((0, 0), (win - 1, 0), (0, 0)))[:, :s]
        count = jnp.minimum(t + 1, win).astype(jnp.float32)[None, :, None]
        pooled.append((hi - lo) / count)
    pooled = jnp.concatenate(pooled, axis=-1).astype(p.dtype) - p
    y = jnp.einsum('bsgd,gde->bsge', pooled.reshape(bsz, s, B_GROUPS, B_HD), w_grp)
    return y.reshape(bsz, s, B_WIDTH) * scale


def mixer_c(bch, conv_w):
    s = bch.shape[1]
    bg, cg, h = jnp.split(bch, 3, axis=-1)
    z = jnp.pad(cg * h, ((0, 0), (CONV_W - 1, 0), (0, 0)))
    conv = sum(conv_w[k] * z[:, k:k + s] for k in range(CONV_W))
    return bg * conv


def rope(x, positions):
    inv_freq = ROPE_THETA ** (-jnp.arange(0, QK_ROPE, 2, dtype=jnp.float32) / QK_ROPE)
    ang = positions.astype(jnp.float32)[..., None] * inv_freq
    cos = jnp.cos(ang)[:, :, None, :].astype(x.dtype)
    sin = jnp.sin(ang)[:, :, None, :].astype(x.dtype)
    x1, x2 = jnp.split(x, 2, axis=-1)
    return jnp.concatenate([x1 * cos - x2 * sin, x2 * cos + x1 * sin], axis=-1)


def mixer_d(cq, ckv, kpe, positions, q_a_norm, w_uq, kv_norm, w_ukv, q_norm_w, k_norm_w):
    bsz, s, _ = cq.shape
    q = (rmsnorm(cq, q_a_norm) @ w_uq).reshape(bsz, s, MLA_HEADS, QK_HD)
    kv = (rmsnorm(ckv, kv_norm) @ w_ukv).reshape(bsz, s, MLA_HEADS, QK_NOPE + V_HD)
    k_nope, v = kv[..., :QK_NOPE], kv[..., QK_NOPE:]
    k_pe = jnp.broadcast_to(kpe[:, :, None, :], (bsz, s, MLA_HEADS, QK_ROPE))
    k = jnp.concatenate([k_nope, k_pe], axis=-1)
    q = rmsnorm(q, q_norm_w)
    k = rmsnorm(k, k_norm_w)
    q = jnp.concatenate([q[..., :QK_NOPE], rope(q[..., QK_NOPE:], positions)], axis=-1)
    k = jnp.concatenate([k[..., :QK_NOPE], rope(k[..., QK_NOPE:], positions)], axis=-1)
    nb = s // Q_BLOCK
    qb = q.reshape(bsz, nb, Q_BLOCK, MLA_HEADS, QK_HD).transpose(1, 0, 2, 3, 4)
    key_pos = jnp.arange(s)
    sm_scale = QK_HD ** -0.5

    def block(args):
        qi, bi = args
        sc = jnp.einsum('bqhd,bkhd->bhqk', qi, k, preferred_element_type=jnp.float32) * sm_scale
        qpos = bi * Q_BLOCK + jnp.arange(Q_BLOCK)
        sc = jnp.where(key_pos[None, :] <= qpos[:, None], sc, -1e30)
        pr = jax.nn.softmax(sc, axis=-1).astype(v.dtype)
        return jnp.einsum('bhqk,bkhd->bqhd', pr, v)

    o = lax.map(block, (qb, jnp.arange(nb)))
    return o.transpose(1, 0, 2, 3, 4).reshape(bsz, s, MLA_HEADS * V_HD)


def moe_swiglu(h, router_w, w_gu, w_down):
    logits = (h @ router_w).astype(jnp.float32)
    top_v, top_i = lax.top_k(logits, TOP_K)
    wts = jax.nn.softmax(top_v, axis=-1)
    gates = jnp.einsum('bsk,bske->bse', wts, jax.nn.one_hot(top_i, N_EXPERTS, dtype=jnp.float32)).astype(h.dtype)
    y = jnp.zeros_like(h)
    for e in range(N_EXPERTS):
        y = y + gates[..., e:e + 1] * swiglu(h, w_gu[e], w_down[e])
    return y


def setup_inputs(seed: int = 0) -> dict:
    key = jax.random.key(seed)
    keys = jax.random.split(key, 40)
    ctr = [0]

    def nrm(shape, scale):
        k = keys[ctr[0]]
        ctr[0] += 1
        return jax.random.normal(k, shape, jnp.float32) * scale

    def gain(shape):
        return 1.0 + nrm(shape, 0.1)

    D = D_MODEL
    return {
        "x": nrm((BATCH, SEQ, D), 1.0),
        "c": nrm((BATCH, D), 1.0),
        "positions": jnp.broadcast_to(jnp.arange(SEQ, dtype=jnp.int32), (BATCH, SEQ)),
        "norm_mix_w": gain((DEPTH, D)),
        "norm_ffn_w": gain((DEPTH, D)),
        "ada_w": nrm((DEPTH, D, 6 * D), D ** -0.5),
        "ada_b": nrm((DEPTH, 6 * D), 0.02),
        "e_w_in": nrm((N_EVEN, D, E_IN), D ** -0.5),
        "a_ln_w": gain((N_EVEN, A_WIDTH)),
        "a_ln_b": nrm((N_EVEN, A_WIDTH), 0.02),
        "a_w_s": nrm((N_EVEN, A_GROUPS, CHUNK, CHUNK), CHUNK ** -0.5),
        "a_b_s": gain((N_EVEN, A_GROUPS, CHUNK)),
        "b_w_grp": nrm((N_EVEN, B_GROUPS, B_HD, B_HD), B_HD ** -0.5),
        "b_scale": gain((N_EVEN, B_WIDTH)),
        "e_w_out": nrm((N_EVEN, E_OUT, D), E_OUT ** -0.5),
        "ffn_w_gu": nrm((N_EVEN, D, 2 * D_FF), D ** -0.5),
        "ffn_w_down": nrm((N_EVEN, D_FF, D), D_FF ** -0.5),
        "o_w_in": nrm((N_ODD, D, O_IN), D ** -0.5),
        "c_conv_w": nrm((N_ODD, CONV_W, C_WIDTH), CONV_W ** -0.5),
        "q_a_norm": gain((N_ODD, Q_LORA)),
        "w_uq": nrm((N_ODD, Q_LORA, MLA_HEADS * QK_HD), Q_LORA ** -0.5),
        "kv_norm": gain((N_ODD, KV_LORA)),
        "w_ukv": nrm((N_ODD, KV_LORA, MLA_HEADS * (QK_NOPE + V_HD)), KV_LORA ** -0.5),
        "q_norm_w": gain((N_ODD, QK_HD)),
        "k_norm_w": gain((N_ODD, QK_HD)),
        "o_w_out": nrm((N_ODD, O_OUT, D), O_OUT ** -0.5),
        "router_w": nrm((N_ODD, D, N_EXPERTS), D ** -0.5),
        "moe_w_gu": nrm((N_ODD, N_EXPERTS, D, 2 * D_FF_EXPERT), D ** -0.5),
        "moe_w_down": nrm((N_ODD, N_EXPERTS, D_FF_EXPERT, D), D_FF_EXPERT ** -0.5),
    }


def reference(x, c, positions, norm_mix_w, norm_ffn_w, ada_w, ada_b,
              e_w_in, a_ln_w, a_ln_b, a_w_s, a_b_s, b_w_grp, b_scale, e_w_out,
              ffn_w_gu, ffn_w_down,
              o_w_in, c_conv_w, q_a_norm, w_uq, kv_norm, w_ukv, q_norm_w, k_norm_w,
              o_w_out, router_w, moe_w_gu, moe_w_down):
    c_act = jax.nn.silu(c)
    for layer in range(DEPTH):
        mod = c_act @ ada_w[layer] + ada_b[layer]
        sh_m, sc_m, g_m, sh_f, sc_f, g_f = [m[:, None, :] for m in jnp.split(mod, 6, axis=-1)]
        h = rmsnorm(x, norm_mix_w[layer]) * (1 + sc_m) + sh_m
        if layer % 2 == 0:
            i = layer // 2
            p = h @ e_w_in[i]
            ya = mixer_a(p[..., :2 * A_WIDTH], a_ln_w[i], a_ln_b[i], a_w_s[i], a_b_s[i])
            yb = mixer_b(p[..., 2 * A_WIDTH:], b_w_grp[i], b_scale[i])
            mix = jnp.concatenate([ya, yb], axis=-1) @ e_w_out[i]
        else:
            i = layer // 2
            p = h @ o_w_in[i]
            o0 = 3 * C_WIDTH
            o1 = o0 + Q_LORA
            o2 = o1 + KV_LORA
            yc = mixer_c(p[..., :o0], c_conv_w[i])
            yd = mixer_d(p[..., o0:o1], p[..., o1:o2], p[..., o2:], positions,
                         q_a_norm[i], w_uq[i], kv_norm[i], w_ukv[i], q_norm_w[i], k_norm_w[i])
            mix = jnp.concatenate([yc, yd], axis=-1) @ o_w_out[i]
        x = x + g_m * mix
        h = rmsnorm(x, norm_ffn_w[layer]) * (1 + sc_f) + sh_f
        if layer % 2 == 0:
            ffn = swiglu(h, ffn_w_gu[layer // 2], ffn_w_down[layer // 2])
        else:
            i = layer // 2
            ffn = moe_swiglu(h, router_w[i], moe_w_gu[i], moe_w_down[i])
        x = x + g_f * ffn
    return x
```

```python
import numpy as np
import concourse.bass as bass
import concourse.mybir as mybir
from concourse.bass_utils import run_bass_kernel_spmd
from contextlib import ExitStack

F32 = mybir.dt.float32
BF16 = mybir.dt.bfloat16
I32 = mybir.dt.int32
AF = mybir.ActivationFunctionType
ALU = mybir.AluOpType

P = 128
D = 1024
KC = 8
T = 512
NT = 4
NTOK = 2048
NCORES = 8
SEQ = 16384
EPS = 1e-6
HALO = 16
D_FF = 2816
D_FFE = 3584
NEXP = 8
POOL_WINDOWS = (2, 4, 8, 16)
QK_HD = 192
SM_SCALE = QK_HD ** -0.5
NEG = -30000.0


class Buf:
    __slots__ = ("t", "w", "r", "dsem", "dcnt", "name")

    def __init__(self, t, name=""):
        self.t = t
        self.w = None
        self.r = {}
        self.dsem = None
        self.dcnt = 0
        self.name = name

    def __getitem__(self, idx):
        return self.t[idx]


class KB:
    def __init__(self, nc, es):
        self.nc = nc
        self.es = es
        self.eng = {"pe": nc.tensor, "act": nc.scalar, "dve": nc.vector, "pool": nc.gpsimd, "sp": nc.sync}
        self.sem = {e: es.enter_context(nc.semaphore("sem_" + e)) for e in ("pe", "act", "dve", "pool")}
        self.cnt = {e: 0 for e in self.sem}
        self.waited = {e: {} for e in self.eng}
        self.semobj = {}
        self.dma_toks = {}
        self.dsem_pool = []
        self.nbuf = 0
        self.pe_self_skip = False

    def sb(self, es, name, shape, dtype):
        self.nbuf += 1
        return Buf(es.enter_context(self.nc.sbuf_tensor(f"{name}_{self.nbuf}", shape, dtype)), name)

    def view(self, t, name=""):
        return Buf(t, name)

    def wait(self, e, tok):
        if tok is None:
            return
        sem, val = tok
        if e == "pe" and self.pe_self_skip and sem is self.sem["pe"]:
            return
        d = self.waited[e]
        if d.get(id(sem), 0) >= val:
            return
        self.eng[e].wait_ge(sem, val)
        d[id(sem)] = val

    def deps(self, e, outs, ins):
        for b in ins:
            self.wait(e, b.w)
        for b in outs:
            self.wait(e, b.w)
            for t in b.r.values():
                self.wait(e, t)

    def done(self, tok, outs, ins):
        for b in outs:
            b.w = tok
            b.r = {}
        for b in ins:
            b.r[id(tok[0])] = tok

    def op(self, e, fn, outs, ins):
        self.deps(e, outs, ins)
        inst = fn(self.eng[e])
        self.cnt[e] += 1
        inst.then_inc(self.sem[e], 1)
        self.done((self.sem[e], self.cnt[e]), outs, ins)

    def pe_begin(self, outs, ins):
        self.deps("pe", outs, ins)

    def pe_end(self, inst, outs, ins):
        self.cnt["pe"] += 1
        inst.then_inc(self.sem["pe"], 1)
        self.done((self.sem["pe"], self.cnt["pe"]), outs, ins)

    def mm(self, out_buf, out_ap, pairs, ins):
        self.pe_begin([out_buf], ins)
        n = len(pairs)
        inst = None
        for i, (l, r) in enumerate(pairs):
            inst = self.nc.tensor.matmul(out_ap, lhsT=l, rhs=r, start=(i == 0), stop=(i == n - 1))
        self.pe_end(inst, [out_buf], ins)

    def _dsem(self, b):
        if b.dsem is None:
            if self.dsem_pool:
                b.dsem = self.dsem_pool.pop()
            else:
                b.dsem = self.es.enter_context(self.nc.semaphore(f"d{len(self.semobj)}"))
            self.semobj[id(b.dsem)] = b.dsem
        return b.dsem

    def dma_in(self, q, dst, dst_ap, src_ap):
        self.deps(q, [dst], [])
        sem = self._dsem(dst)
        self.eng[q].dma_start(out=dst_ap, in_=src_ap).then_inc(sem, 16)
        dst.dcnt += 16
        tok = (sem, dst.dcnt)
        dst.w = tok
        dst.r = {}
        self.dma_toks[id(sem)] = tok

    def dma_out(self, q, dst_ap, src, src_ap):
        self.deps(q, [], [src])
        sem = self._dsem(src)
        self.eng[q].dma_start(out=dst_ap, in_=src_ap).then_inc(sem, 16)
        src.dcnt += 16
        tok = (sem, src.dcnt)
        src.r[id(sem)] = tok
        self.dma_toks[id(sem)] = tok

    def barrier(self):
        toks = [(self.sem[e], self.cnt[e]) for e in self.sem if self.cnt[e] > 0] + list(self.dma_toks.values())
        for e in self.eng:
            for t in toks:
                self.wait(e, t)

    def final_wait(self):
        for t in self.dma_toks.values():
            self.wait("sp", t)


def bcast_free(ap, n):
    return ap.to_broadcast([P, n])


def build_program(phase):
    nc = bass.Bass("TRN2", target_bir_lowering=False)
    dram = {}

    def din(name, shape, dtype=F32):
        dram[name] = nc.dram_tensor(name, list(shape), dtype, kind="ExternalInput").ap()
        return dram[name]

    def dout(name, shape, dtype=F32):
        dram[name] = nc.dram_tensor(name, list(shape), dtype, kind="ExternalOutput").ap()
        return dram[name]

    with ExitStack() as es:
        kb = KB(nc, es)
        kb.pe_self_skip = (phase == 2)
        ident = kb.sb(es, "ident", [P, P], F32)
        identb = kb.sb(es, "identb", [P, P], BF16)
        ones_b = kb.sb(es, "ones_b", [P, P], BF16)
        cst = kb.sb(es, "cst", [P, 16], F32)
        featA = kb.sb(es, "featA", [P, 128], F32)
        featB = kb.sb(es, "featB", [P, 32], F32)
        modT = kb.sb(es, "modT", [P, 96], F32)
        scl = kb.sb(es, "scl", [P, 32], F32)
        xT = kb.sb(es, "xT", [P, KC, NTOK], F32)
        ps = [Buf(es.enter_context(nc.psum_tensor(f"ps{i}", [P, 512], F32)), f"ps{i}") for i in range(8)]

        d_ident = din("ident", [P, P])
        d_cst = din("cst", [P, 16])
        d_vecA = din("vecA", [P, P])
        d_vecB = din("vecB", [32, P])
        d_c = din("cT", [P, KC])
        d_ada_w = din("ada_w", [2, D, 6 * D])

        kb.dma_in("sp", ident, ident[:], d_ident[:, :])
        kb.dma_in("sp", cst, cst[:], d_cst[:, :])
        kb.op("dve", lambda e: e.tensor_copy(out=identb[:], in_=ident[:]), [identb], [ident])
        kb.op("dve", lambda e: e.memset(ones_b[:], 1.0), [ones_b], [])
        eps_ap = cst[:, 3:4]

        def small_vectors(es2):
            stA = kb.sb(es2, "stA", [P, P], F32)
            stB = kb.sb(es2, "stB", [32, P], F32)
            kb.dma_in("sp", stA, stA[:], d_vecA[:, :])
            kb.dma_in("sp", stB, stB[:], d_vecB[:, :])
            kb.pe_begin([ps[0]], [stA, ident])
            i1 = nc.tensor.transpose(ps[0][:, 0:128], stA[:], ident[:])
            kb.pe_end(i1, [ps[0]], [stA, ident])
            kb.op("dve", lambda e: e.tensor_copy(out=featA[:], in_=ps[0][:, 0:128]), [featA], [ps[0]])
            kb.pe_begin([ps[1]], [stB, ident])
            i2 = nc.tensor.transpose(ps[1][:, 0:32], stB[:], ident[0:32, 0:32])
            kb.pe_end(i2, [ps[1]], [stB, ident])
            kb.op("dve", lambda e: e.tensor_copy(out=featB[:], in_=ps[1][:, 0:32]), [featB], [ps[1]])

        def mod_vectors(es2):
            cT = kb.sb(es2, "cT", [P, KC], F32)
            cb = kb.sb(es2, "cb", [P, KC], BF16)
            kb.dma_in("sp", cT, cT[:], d_c[:, :])
            kb.op("act", lambda e: e.activation(out=cb[:], in_=cT[:], func=AF.Silu), [cb], [cT])
            wa = [kb.sb(es2, f"wa{i}", [P, KC, 1536], BF16) for i in range(2)]
            i = 0
            for l in range(2):
                for q in range(4):
                    w = wa[i % 2]
                    i += 1
                    src = d_ada_w[l].rearrange("(k p) f -> p k f", p=P)
                    for h in range(2):
                        kb.dma_in("pool", w, w[:, 4 * h:4 * h + 4, :], src[:, 4 * h:4 * h + 4, q * 1536:(q + 1) * 1536])
                    bank = ps[2 + (i % 2)]
                    kb.pe_begin([bank], [w, cb])
                    inst = None
                    for m in range(12):
                        for k in range(KC):
                            inst = nc.tensor.matmul(bank[:, m:m + 1], lhsT=w[:, k, m * 128:(m + 1) * 128],
                                                    rhs=cb[:, k:k + 1], start=(k == 0), stop=(k == KC - 1))
                    kb.pe_end(inst, [bank], [w, cb])
                    c0 = l * 48 + q * 12
                    a0 = l * 64 + 16 + q * 12
                    kb.op("dve", lambda e, bank=bank, c0=c0, a0=a0: e.tensor_tensor(
                        out=modT[:, c0:c0 + 12], in0=bank[:, 0:12], in1=featA[:, a0:a0 + 12], op=ALU.add),
                        [modT], [bank, featA])
            for l in range(2):
                for n in range(2):
                    sc0 = l * 48 + (8 if n == 0 else 32)
                    nw0 = l * 64 + n * 8
                    o0 = l * 16 + n * 8
                    kb.op("dve", lambda e, sc0=sc0, nw0=nw0, o0=o0: e.scalar_tensor_tensor(
                        out=scl[:, o0:o0 + 8], in0=modT[:, sc0:sc0 + 8], scalar=1.0, in1=featA[:, nw0:nw0 + 8],
                        op0=ALU.add, op1=ALU.mult), [scl], [modT, featA])

        with ExitStack() as es2:
            small_vectors(es2)
            mod_vectors(es2)
            kb.barrier()

        def rms_adaln(es_tmp, src_fn, dst_fn, ncols, l, n, tmps, h32_cb=None):
            sq, rs, rstd, tmp2, bankS = tmps
            for k in range(KC):
                sb_, sap = src_fn(k)
                kb.op("act", lambda e, k=k, sap=sap: e.activation(out=sq[:, k, 0:ncols], in_=sap, func=AF.Square),
                      [sq], [sb_])
            kb.mm(bankS, bankS[:, 0:ncols], [(ones_b[:], sq[:, k, 0:ncols]) for k in range(KC)], [ones_b, sq])
            kb.op("act", lambda e: e.activation(out=rs[:, 0:ncols], in_=bankS[:, 0:ncols], func=AF.Sqrt,
                                                bias=eps_ap, scale=1.0 / D), [rs], [bankS, cst])
            kb.op("dve", lambda e: e.reciprocal(out=rstd[:, 0:ncols], in_=rs[:, 0:ncols]), [rstd], [rs])
            for k in range(KC):
                sb_, sap = src_fn(k)
                db_, dap = dst_fn(k)
                tb = tmp2[k % 2]
                sc_ap = scl[:, l * 16 + n * 8 + k:l * 16 + n * 8 + k + 1]
                sh_ap = modT[:, l * 48 + (0 if n == 0 else 24) + k:l * 48 + (0 if n == 0 else 24) + k + 1]
                kb.op("dve", lambda e, sap=sap, sc_ap=sc_ap, tb=tb: e.scalar_tensor_tensor(
                    out=tb[:, 0:ncols], in0=sap, scalar=sc_ap, in1=rstd[:, 0:ncols], op0=ALU.mult, op1=ALU.mult),
                    [tb], [sb_, scl, rstd])
                if h32_cb is None:
                    kb.op("act", lambda e, dap=dap, tb=tb, sh_ap=sh_ap: e.activation(
                        out=dap, in_=tb[:, 0:ncols], func=AF.Identity, bias=sh_ap, scale=1.0), [db_], [tb, modT])
                else:
                    kb.op("act", lambda e, tb=tb, sh_ap=sh_ap: e.activation(
                        out=tb[:, 0:ncols], in_=tb[:, 0:ncols], func=AF.Identity, bias=sh_ap, scale=1.0), [tb], [tb, modT])
                    kb.op("dve", lambda e, dap=dap, tb=tb: e.tensor_copy(out=dap, in_=tb[:, 0:ncols]), [db_], [tb])
                    h32_cb(k, tb)

        def norm_tmps(es_tmp, bankS):
            sq = kb.sb(es_tmp, "sq", [P, KC, T], BF16)
            rs = kb.sb(es_tmp, "rs", [P, T], F32)
            rstd = kb.sb(es_tmp, "rstd", [P, T], F32)
            tmp2 = [kb.sb(es_tmp, f"ntmp{i}", [P, T], F32) for i in range(2)]
            return (sq, rs, rstd, tmp2, bankS)

        def load_w(q, dst, dst_ap_fn, src3, nk, c0, c1):
            half = max(1, nk // 2)
            for k0 in range(0, nk, half):
                kb.dma_in(q, dst, dst_ap_fn(k0, min(nk, k0 + half)), src3[:, k0:min(nk, k0 + half), c0:c1])

        def ffn_blocks(hT, blocks, gf_col0, ring):
            wgu, wdn, sg, act = ring

            def issue_load(i):
                bl = blocks[i]
                s_ = i % 2
                wg_, wd_ = wgu[s_], wdn[s_]
                fb = bl["fb"]
                nch = fb // 128
                load_w("pool", wg_, lambda a_, b_: wg_[:, a_:b_, 0, 0:fb], bl["w_gu3"], KC, bl["gcol"] + bl["f0"], bl["gcol"] + bl["f0"] + fb)
                load_w("pool", wg_, lambda a_, b_: wg_[:, a_:b_, 1, 0:fb], bl["w_gu3"], KC, bl["ucol"] + bl["f0"], bl["ucol"] + bl["f0"] + fb)
                r0 = bl["dn_row0"] + bl["f0"]
                src_dn = bl["w_dn"][r0:r0 + fb, :].rearrange("(c p) d -> p c d", p=P)
                load_w("pool", wd_, lambda a_, b_: wd_[:, a_:b_, :], src_dn, nch, 0, D)

            pending = [None]
            issue_load(0)
            for i, bl in enumerate(blocks):
                s_ = i % 2
                wg_, wd_ = wgu[s_], wdn[s_]
                fb = bl["fb"]
                nch = fb // 128
                gate_fn = bl["gate_fn"]
                for j in range(NT):
                    cols = slice(j * T, (j + 1) * T)
                    for c in range(nch):
                        bg_, bu_ = ps[(c % 2) * 2], ps[(c % 2) * 2 + 1]
                        kb.mm(bg_, bg_[:, :], [(wg_[:, k, 0, c * 128:(c + 1) * 128], hT[:, k, cols]) for k in range(KC)], [wg_, hT])
                        kb.mm(bu_, bu_[:, :], [(wg_[:, k, 1, c * 128:(c + 1) * 128], hT[:, k, cols]) for k in range(KC)], [wg_, hT])
                        sgb = sg[c % 2]
                        kb.op("act", lambda e, sgb=sgb, bg_=bg_: e.activation(out=sgb[:], in_=bg_[:, :], func=AF.Silu), [sgb], [bg_])
                        ab = act[(j % 2) * 4 + c]
                        if gate_fn is None:
                            kb.op("dve", lambda e, ab=ab, sgb=sgb, bu_=bu_: e.tensor_tensor(
                                out=ab[:], in0=bu_[:, :], in1=sgb[:], op=ALU.mult), [ab], [bu_, sgb])
                        else:
                            gb, gap = gate_fn(j)
                            kb.op("dve", lambda e, sgb=sgb, bu_=bu_: e.tensor_tensor(
                                out=sgb[:], in0=bu_[:, :], in1=sgb[:], op=ALU.mult), [sgb], [bu_, sgb])
                            kb.op("dve", lambda e, ab=ab, sgb=sgb, gap=gap: e.tensor_tensor(
                                out=ab[:], in0=sgb[:], in1=gap, op=ALU.mult), [ab], [sgb, gb])

                    def down(j=j, cols=cols, wd_=wd_, nch=nch):
                        for n in range(KC):
                            bo = ps[4 + (n % 4)]
                            kb.mm(bo, bo[:, :], [(wd_[:, c, n * 128:(n + 1) * 128], act[(j % 2) * 4 + c][:]) for c in range(nch)],
                                  [wd_] + [act[(j % 2) * 4 + c] for c in range(nch)])
                            gf = modT[:, gf_col0 + n:gf_col0 + n + 1]
                            kb.op("dve", lambda e, bo=bo, n=n, gf=gf: e.scalar_tensor_tensor(
                                out=xT[:, n, cols], in0=bo[:, :], scalar=gf, in1=xT[:, n, cols], op0=ALU.mult, op1=ALU.add),
                                [xT], [bo, modT, xT])
                    if pending[0] is not None:
                        pending[0]()
                    pending[0] = down
                    if j == 0 and i + 1 < len(blocks):
                        issue_load(i + 1)
            if pending[0] is not None:
                pending[0]()

        def make_blocks(w_gu3, w_dn, dff, gcol, ucol, dn_row0, gate_fn):
            out = []
            for f0 in range(0, dff, 512):
                out.append(dict(w_gu3=w_gu3, w_dn=w_dn, gcol=gcol, ucol=ucol, dn_row0=dn_row0, f0=f0, fb=min(512, dff - f0), gate_fn=gate_fn))
            return out

        def ffn_ring(es3):
            wgu = [kb.sb(es3, f"wgu{i}", [P, KC, 2, 512], BF16) for i in range(2)]
            wdn = [kb.sb(es3, f"wdn{i}", [P, 4, D], BF16) for i in range(2)]
            sg = [kb.sb(es3, f"sg{i}", [P, T], BF16) for i in range(2)]
            act = [kb.sb(es3, f"act{i}", [P, T], BF16) for i in range(8)]
            return (wgu, wdn, sg, act)

        if phase == 1:
            build_phase1(nc, kb, es, din, dout, locals())
        else:
            build_phase2(nc, kb, es, din, dout, locals())
        kb.final_wait()
    return nc


def build_phase1(nc, kb, es, din, dout, env):
    ps, xT, ident, identb, ones_b, cst, featA, featB, modT, scl = (env[k] for k in (
        "ps", "xT", "ident", "identb", "ones_b", "cst", "featA", "featB", "modT", "scl"))
    rms_adaln, norm_tmps, load_w, ffn_blocks, make_blocks, ffn_ring = (env[k] for k in (
        "rms_adaln", "norm_tmps", "load_w", "ffn_blocks", "make_blocks", "ffn_ring"))
    eps_ap = cst[:, 3:4]

    d_x = din("x", [NTOK, D])
    d_xh = din("xh", [NT * HALO, D])
    d_hflag = din("hflag", [P, NT])
    d_corr = din("corr", [P, 4, HALO])
    d_e_w_in = din("e_w_in", [D, 1536])
    d_e_w_out = din("e_w_out", [D, D])
    d_lnwb = din("lnwb", [P, 2, 512])
    d_bsb = din("bsb", [P, 4, 512])
    d_wsT = din("wsT", [4, P, P])
    d_trim = din("trimask", [P, P])
    d_wgrp = din("b_w_grp", [4, P, P])
    d_ffn_gu = din("ffn_w_gu", [D, 2 * D_FF])
    d_ffn_dn = din("ffn_w_down", [D_FF, D])
    d_o_w_in = din("o_w_in", [D, 2112])
    d_w_kpe = din("w_kpe", [D, P])
    d_w_uq_n = din("w_uq_n", [256, 4 * P])
    d_w_uq_r = din("w_uq_r", [256, 4 * P])
    d_w_ukv = din("w_ukv", [256, 4 * 256])
    d_pos = din("posb", [P, NTOK], I32)
    o_x1T = dout("x1T", [P, KC, NTOK])
    o_QT = dout("QT", [P, 8, NTOK], BF16)
    o_KT = dout("KT", [P, 8, NTOK], BF16)
    o_V = dout("V", [P, 16, 512], BF16)
    o_z = dout("zT", [P, 4, NTOK], BF16)
    o_bg = dout("bgT", [P, 4, NTOK], BF16)

    xhT = kb.sb(es, "xhT", [P, KC, NT * HALO], F32)

    with ExitStack() as e1:
        stg = [kb.sb(e1, f"xstg{i}", [P, D], F32) for i in range(8)]
        hst = kb.sb(e1, "hst", [NT * HALO, D], F32)
        kb.dma_in("sp", hst, hst[:], d_xh[:, :])
        for j in range(NT):
            for b in range(4):
                s = stg[(j % 2) * 4 + b]
                kb.dma_in("sp", s, s[:], d_x[(j * 4 + b) * 128:(j * 4 + b + 1) * 128, :])
            for k in range(KC):
                bank = ps[k % 4]
                srcs = [stg[(j % 2) * 4 + b] for b in range(4)]
                kb.pe_begin([bank], srcs + [ident])
                inst = None
                for b in range(4):
                    inst = nc.tensor.transpose(bank[:, b * 128:(b + 1) * 128], srcs[b][:, k * 128:(k + 1) * 128], ident[:])
                kb.pe_end(inst, [bank], srcs + [ident])
                eng = "act" if k % 2 == 0 else "dve"
                if eng == "act":
                    kb.op("act", lambda e, bank=bank, k=k, j=j: e.activation(out=xT[:, k, j * T:(j + 1) * T], in_=bank[:, :], func=AF.Copy), [xT], [bank])
                else:
                    kb.op("dve", lambda e, bank=bank, k=k, j=j: e.tensor_copy(out=xT[:, k, j * T:(j + 1) * T], in_=bank[:, :]), [xT], [bank])
        bank = ps[4]
        kb.pe_begin([bank], [hst, ident])
        inst = None
        for k in range(KC):
            inst = nc.tensor.transpose(bank[:, k * 64:(k + 1) * 64], hst[:, k * 128:(k + 1) * 128], ident[0:64, 0:64])
        kb.pe_end(inst, [bank], [hst, ident])
        kb.op("dve", lambda e: e.tensor_copy(out=xhT[:].rearrange("p k c -> p (k c)"), in_=bank[:, :]), [xhT], [bank])
        kb.barrier()

    with ExitStack() as e2:
        w_in = kb.sb(e2, "w_in", [P, KC, 1536], BF16)
        w_out = kb.sb(e2, "w_out", [P, KC, D], BF16)
        src_in = d_e_w_in.rearrange("(k p) f -> p k f", p=P)
        for q in range(3):
            load_w("pool", w_in, lambda a, b, q=q: w_in[:, a:b, q * 512:(q + 1) * 512], src_in, KC, q * 512, (q + 1) * 512)
        src_out = d_e_w_out.rearrange("(k p) f -> p k f", p=P)
        for q in range(2):
            load_w("pool", w_out, lambda a, b, q=q: w_out[:, a:b, q * 512:(q + 1) * 512], src_out, KC, q * 512, (q + 1) * 512)
        lnwb = kb.sb(e2, "lnwb", [P, 2, 512], F32)
        bsb = kb.sb(e2, "bsb", [P, 4, 512], F32)
        hflag = kb.sb(e2, "hflag", [P, NT], F32)
        corr = kb.sb(e2, "corr", [P, 4, HALO], F32)
        wsT32 = kb.sb(e2, "wsT32", [P, 4, P], F32)
        trim = kb.sb(e2, "trim", [P, P], F32)
        wsT = kb.sb(e2, "wsT", [P, 4, P], BF16)
        wgrp = kb.sb(e2, "wgrp", [P, 4, P], BF16)
        kb.dma_in("sp", lnwb, lnwb[:], d_lnwb[:, :, :])
        kb.dma_in("sp", bsb, bsb[:], d_bsb[:, :, :])
        kb.dma_in("sp", hflag, hflag[:], d_hflag[:, :])
        kb.dma_in("sp", corr, corr[:], d_corr[:, :, :])
        kb.dma_in("sp", wsT32, wsT32[:], d_wsT.rearrange("g s t -> s g t"))
        kb.dma_in("sp", trim, trim[:], d_trim[:, :])
        kb.dma_in("pool", wgrp, wgrp[:], d_wgrp.rearrange("g d e -> d g e"))
        for g in range(4):
            kb.op("dve", lambda e, g=g: e.tensor_tensor(out=wsT[:, g, :], in0=wsT32[:, g, :], in1=trim[:], op=ALU.mult), [wsT], [wsT32, trim])

        hT = kb.sb(e2, "hT0", [P, KC, T], BF16)
        hhT = kb.sb(e2, "hhT", [P, KC, NT * HALO], BF16)
        tmps = norm_tmps(e2, ps[7])
        uT = kb.sb(e2, "uT", [P, 4, T], F32)
        yT = kb.sb(e2, "yT", [P, KC, T], BF16)
        g_sq = [kb.sb(e2, f"g_sq{i}", [P, T], F32) for i in range(2)]
        g_t = [kb.sb(e2, f"g_t{i}", [P, T], F32) for i in range(2)]
        vt = [kb.sb(e2, f"vt{i}", [P, T], F32) for i in range(2)]
        vln = [kb.sb(e2, f"vln{i}", [P, T], BF16) for i in range(4)]
        bst = kb.sb(e2, "bst", [P, 8], F32)
        mv = kb.sb(e2, "mv", [P, 4], F32)
        pbx = [kb.sb(e2, f"pbx{i}", [P, HALO + T], F32) for i in range(2)]
        pw = [kb.sb(e2, f"pw{i}", [P, HALO + T], F32) for i in range(2)]
        pooled = [kb.sb(e2, f"pooled{i}", [P, T], BF16) for i in range(2)]
        svt = kb.sb(e2, "svt", [P, T], F32)

        rms_adaln(e2, lambda k: (xhT, xhT[:, k, :]), lambda k: (hhT, hhT[:, k, :]), NT * HALO, 0, 0, tmps)

        def gelu_tanh(bank, out_buf, out_ap, i):
            sqb, tb = g_sq[i % 2], g_t[i % 2]
            kb.op("act", lambda e: e.activation(out=sqb[:], in_=bank[:, :], func=AF.Square), [sqb], [bank])
            kb.op("dve", lambda e: e.tensor_scalar(out=sqb[:], in0=sqb[:], scalar1=0.044715, scalar2=1.0, op0=ALU.mult, op1=ALU.add), [sqb], [sqb])
            kb.op("dve", lambda e: e.tensor_tensor(out=tb[:], in0=bank[:, :], in1=sqb[:], op=ALU.mult), [tb], [bank, sqb])
            kb.op("act", lambda e: e.activation(out=tb[:], in_=tb[:], func=AF.Sigmoid, scale=1.5957691216057308), [tb], [tb])
            kb.op("dve", lambda e: e.tensor_tensor(out=out_ap, in0=bank[:, :], in1=tb[:], op=ALU.mult), [out_buf], [bank, tb])

        gi = 0
        for j in range(NT):
            cols = slice(j * T, (j + 1) * T)
            rms_adaln(e2, lambda k: (xT, xT[:, k, cols]), lambda k: (hT, hT[:, k, :]), T, 0, 0, tmps)
            for m in range(4):
                bank = ps[m % 2]
                kb.mm(bank, bank[:, :], [(w_in[:, k, m * 128:(m + 1) * 128], hT[:, k, :]) for k in range(KC)], [w_in, hT])
                gelu_tanh(bank, uT, uT[:, m, :], gi)
                gi += 1
            for b in range(4):
                bank = ps[2 + (b % 2)]
                kb.mm(bank, bank[:, :], [(hT[:, k, b * 128:(b + 1) * 128], w_in[:, k, 512:1024]) for k in range(KC)], [w_in, hT])
                v = vt[b % 2]
                gelu_tanh(bank, v, v[:], gi)
                gi += 1
                kb.op("dve", lambda e, v=v: e.bn_stats(out=bst[:, 0:6], in_=v[:]), [bst], [v])
                kb.op("dve", lambda e: e.bn_aggr(out=mv[:, 0:2], in_=bst[:, 0:6]), [mv], [bst])
                kb.op("act", lambda e: e.activation(out=mv[:, 2:3], in_=mv[:, 1:2], func=AF.Sqrt, bias=eps_ap, scale=1.0), [mv], [mv, cst])
                kb.op("dve", lambda e: e.reciprocal(out=mv[:, 3:4], in_=mv[:, 2:3]), [mv], [mv])
                kb.op("dve", lambda e, v=v: e.tensor_scalar(out=v[:], in0=v[:], scalar1=mv[:, 0:1], scalar2=mv[:, 3:4],
                                                             op0=ALU.subtract, op1=ALU.mult), [v], [v, mv])
                kb.op("dve", lambda e, v=v: e.tensor_tensor(out=v[:], in0=v[:], in1=lnwb[:, 0, :], op=ALU.mult), [v], [v, lnwb])
                kb.op("dve", lambda e, v=v, b=b: e.tensor_tensor(out=vln[b][:], in0=v[:], in1=lnwb[:, 1, :], op=ALU.add), [vln[b]], [v, lnwb])
            for g in range(4):
                bank = ps[4 + (g % 2)]
                kb.pe_begin([bank], vln + [wsT])
                inst = None
                for b in range(4):
                    inst = nc.tensor.matmul(bank[:, b * 128:(b + 1) * 128], lhsT=vln[b][:, g * 128:(g + 1) * 128],
                                            rhs=wsT[:, g, :], start=True, stop=True)
                kb.pe_end(inst, [bank], vln + [wsT])
                kb.op("dve", lambda e, bank=bank, g=g: e.tensor_tensor(out=svt[:], in0=bank[:, :], in1=bsb[:, g, :], op=ALU.add), [svt], [bank, bsb])
                kb.op("dve", lambda e, g=g: e.tensor_tensor(out=yT[:, g, :], in0=svt[:], in1=uT[:, g, :], op=ALU.mult), [yT], [svt, uT])
            for m in range(4):
                bank = ps[6]
                hb = ps[4 + (m % 2)]
                px = pbx[m % 2]
                kb.mm(bank, bank[:, :], [(w_in[:, k, 1024 + m * 128:1024 + (m + 1) * 128], hT[:, k, :]) for k in range(KC)], [w_in, hT])
                kb.mm(hb, hb[:, 0:HALO], [(w_in[:, k, 1024 + m * 128:1024 + (m + 1) * 128], hhT[:, k, j * HALO:(j + 1) * HALO]) for k in range(KC)], [w_in, hhT])
                kb.op("act", lambda e, px=px, bank=bank: e.activation(out=px[:, HALO:], in_=bank[:, :], func=AF.Copy), [px], [bank])
                kb.op("dve", lambda e, px=px, hb=hb, j=j: e.tensor_scalar(out=px[:, 0:HALO], in0=hb[:, 0:HALO], scalar1=hflag[:, j:j + 1],
                                                                          scalar2=None, op0=ALU.mult), [px], [hb, hflag])
                win = POOL_WINDOWS[m]
                cur = px
                sh = 1
                step = 0
                W = HALO + T
                while sh < win:
                    nxt = pw[step % 2]
                    kb.op("dve", lambda e, cur=cur, nxt=nxt, sh=sh: e.tensor_tensor(
                        out=nxt[:, sh:W], in0=cur[:, sh:W], in1=cur[:, 0:W - sh], op=ALU.add), [nxt], [cur])
                    kb.op("dve", lambda e, cur=cur, nxt=nxt, sh=sh: e.tensor_copy(out=nxt[:, 0:sh], in_=cur[:, 0:sh]), [nxt], [cur])
                    cur = nxt
                    sh *= 2
                    step += 1
                if j == 0:
                    kb.op("dve", lambda e, cur=cur, m=m: e.tensor_tensor(out=cur[:, HALO:2 * HALO], in0=cur[:, HALO:2 * HALO],
                                                                         in1=corr[:, m, :], op=ALU.mult), [cur], [cur, corr])
                pl = pooled[m % 2]
                kb.op("dve", lambda e, cur=cur, px=px, pl=pl, win=win: e.scalar_tensor_tensor(
                    out=pl[:], in0=cur[:, HALO:], scalar=1.0 / win, in1=px[:, HALO:], op0=ALU.mult, op1=ALU.subtract), [pl], [cur, px])
                bank2 = ps[m % 2]
                kb.mm(bank2, bank2[:, :], [(wgrp[:, m, :], pl[:])], [wgrp, pl])
                kb.op("act", lambda e, bank2=bank2, m=m: e.activation(out=yT[:, 4 + m, :], in_=bank2[:, :], func=AF.Copy,
                                                                      scale=featB[:, m:m + 1]), [yT], [bank2, featB])
            for n in range(KC):
                bank = ps[2 + (n % 2)]
                kb.mm(bank, bank[:, :], [(w_out[:, k, n * 128:(n + 1) * 128], yT[:, k, :]) for k in range(KC)], [w_out, yT])
                gm = modT[:, 16 + n:17 + n]
                kb.op("dve", lambda e, bank=bank, n=n, gm=gm: e.scalar_tensor_tensor(
                    out=xT[:, n, cols], in0=bank[:, :], scalar=gm, in1=xT[:, n, cols], op0=ALU.mult, op1=ALU.add), [xT], [bank, modT, xT])
        kb.barrier()

    with ExitStack() as e3:
        hT = kb.sb(e3, "hTf", [P, KC, NTOK], BF16)
        with ExitStack() as e3a:
            tmps = norm_tmps(e3a, ps[7])
            for j in range(NT):
                cols = slice(j * T, (j + 1) * T)
                rms_adaln(e3a, lambda k: (xT, xT[:, k, cols]), lambda k: (hT, hT[:, k, cols]), T, 0, 1, tmps)
            kb.barrier()
        ring = ffn_ring(e3)
        w_gu3 = d_ffn_gu.rearrange("(k p) f -> p k f", p=P)
        ffn_blocks(hT, make_blocks(w_gu3, d_ffn_dn, D_FF, 0, D_FF, 0, None), 40, ring)
        kb.barrier()

    for k in range(KC):
        kb.dma_out("sp", o_x1T[:, k, :], xT, xT[:, k, :])

    with ExitStack() as e4:
        hT = kb.sb(e4, "hT1", [P, KC, T], BF16)
        tmps = norm_tmps(e4, ps[7])
        sq, rs_, rstd_, tmp2_, _ = tmps
        w1 = kb.sb(e4, "w1", [P, KC, 2112 + 128], BF16)
        src1 = d_o_w_in.rearrange("(k p) f -> p k f", p=P)
        for q in range(4):
            load_w("pool", w1, lambda a, b, q=q: w1[:, a:b, q * 528:(q + 1) * 528], src1, KC, q * 528, (q + 1) * 528)
        srck = d_w_kpe.rearrange("(k p) f -> p k f", p=P)
        load_w("pool", w1, lambda a, b: w1[:, a:b, 2112:2240], srck, KC, 0, 128)
        wq_n = kb.sb(e4, "wq_n", [P, 2, 512], BF16)
        wq_r = kb.sb(e4, "wq_r", [P, 2, 512], BF16)
        wkv = kb.sb(e4, "wkv", [P, 2, 1024], BF16)
        kb.dma_in("pool", wq_n, wq_n[:], d_w_uq_n.rearrange("(k p) f -> p k f", p=P))
        kb.dma_in("pool", wq_r, wq_r[:], d_w_uq_r.rearrange("(k p) f -> p k f", p=P))
        kb.dma_in("pool", wkv, wkv[:], d_w_ukv.rearrange("(k p) f -> p k f", p=P))
        trig = kb.sb(e4, "trig", [P, NTOK], F32)
        with ExitStack() as e4b:
            posi = kb.sb(e4b, "posi", [P, NTOK], I32)
            ang = kb.sb(e4b, "ang", [P, NTOK], F32)
            kq = kb.sb(e4b, "kq", [P, NTOK], F32)
            kb.dma_in("sp", posi, posi[:], d_pos[:, :])
            kb.op("dve", lambda e: e.tensor_copy(out=ang[:], in_=posi[:]), [ang], [posi])
            kb.op("dve", lambda e: e.tensor_scalar(out=ang[:], in0=ang[:], scalar1=cst[:, 0:1], scalar2=None, op0=ALU.mult), [ang], [ang, cst])
            MAGIC = 12582912.0
            kb.op("dve", lambda e: e.tensor_scalar(out=kq[:], in0=ang[:], scalar1=0.15915494309189535, scalar2=MAGIC, op0=ALU.mult, op1=ALU.add), [kq], [ang])
            kb.op("dve", lambda e: e.tensor_scalar(out=kq[:], in0=kq[:], scalar1=MAGIC, scalar2=None, op0=ALU.subtract), [kq], [kq])
            kb.op("dve", lambda e: e.scalar_tensor_tensor(out=ang[:], in0=kq[:], scalar=-6.28125, in1=ang[:], op0=ALU.mult, op1=ALU.add), [ang], [kq, ang])
            kb.op("dve", lambda e: e.scalar_tensor_tensor(out=ang[:], in0=kq[:], scalar=-0.0019353071795864769, in1=ang[:], op0=ALU.mult, op1=ALU.add), [ang], [kq, ang])
            kb.op("dve", lambda e: e.tensor_scalar(out=ang[:], in0=ang[:], scalar1=3.1415925, scalar2=-3.1415925, op0=ALU.min, op1=ALU.max), [ang], [ang])
            kb.op("dve", lambda e: e.tensor_scalar(out=kq[0:64, :], in0=ang[0:64, :], scalar1=-1.0, scalar2=None, op0=ALU.mult), [kq], [ang])
            kb.op("dve", lambda e: e.tensor_tensor(out=ang[0:64, :], in0=ang[0:64, :], in1=kq[0:64, :], op=ALU.max), [ang], [ang, kq])
            kb.op("act", lambda e: e.activation(out=trig[0:64, :], in_=ang[0:64, :], func=AF.Sin, bias=cst[0:64, 2:3], scale=cst[0:64, 1:2]), [trig], [ang, cst])
            kb.op("act", lambda e: e.activation(out=trig[64:128, :], in_=ang[64:128, :], func=AF.Sin, bias=cst[64:128, 2:3], scale=cst[64:128, 1:2]), [trig], [ang, cst])
            kb.barrier()

        zT = kb.sb(e4, "zT", [P, 4, T], BF16)
        bgT = kb.sb(e4, "bgT", [P, 4, T], BF16)
        cgs = [kb.sb(e4, f"cgs{i}", [P, T], F32) for i in range(2)]
        lat = kb.sb(e4, "lat", [P, 4, T], F32)
        latn = kb.sb(e4, "latn", [P, 4, T], BF16)
        lrstd = [kb.sb(e4, f"lrstd{i}", [P, T], F32) for i in range(2)]
        kpe_raw = kb.sb(e4, "kpe_raw", [P, T], F32)
        lrs = rs_
        hrs = rs_
        hrstd = rstd_
        ur = tmp2_[0]
        urs = tmp2_[1]
        QTt = kb.sb(e4, "QTt", [P, 8, T], BF16)
        KTt = kb.sb(e4, "KTt", [P, 8, T], BF16)
        Vt = kb.sb(e4, "Vt", [P, 4, 512], BF16)
        kb.op("dve", lambda e: e.memset(QTt[:], 0.0), [QTt], [])
        kb.op("dve", lambda e: e.memset(KTt[:], 0.0), [KTt], [])

        for j in range(NT):
            cols = slice(j * T, (j + 1) * T)
            rms_adaln(e4, lambda k: (xT, xT[:, k, cols]), lambda k: (hT, hT[:, k, :]), T, 1, 0, tmps)
            hj = lambda k: hT[:, k, :]
            for m in range(4):
                bank = ps[m % 2]
                kb.mm(bank, bank[:, :], [(w1[:, k, m * 128:(m + 1) * 128], hj(k)) for k in range(KC)], [w1, hT])
                kb.op("act", lambda e, bank=bank, m=m: e.activation(out=bgT[:, m, :], in_=bank[:, :], func=AF.Copy), [bgT], [bank])
                b2 = ps[2 + (m % 2)]
                kb.mm(b2, b2[:, :], [(w1[:, k, 512 + m * 128:512 + (m + 1) * 128], hj(k)) for k in range(KC)], [w1, hT])
                cg = cgs[m % 2]
                kb.op("act", lambda e, b2=b2, cg=cg: e.activation(out=cg[:], in_=b2[:, :], func=AF.Copy), [cg], [b2])
                b3 = ps[4 + (m % 2)]
                kb.mm(b3, b3[:, :], [(w1[:, k, 1024 + m * 128:1024 + (m + 1) * 128], hj(k)) for k in range(KC)], [w1, hT])
                kb.op("dve", lambda e, b3=b3, cg=cg, m=m: e.tensor_tensor(out=zT[:, m, :], in0=b3[:, :], in1=cg[:], op=ALU.mult), [zT], [b3, cg])
            kb.dma_out("sp", o_z[:, :, cols], zT, zT[:])
            kb.dma_out("sp", o_bg[:, :, cols], bgT, bgT[:])
            for m in range(4):
                bank = ps[m % 2]
                kb.mm(bank, bank[:, :], [(w1[:, k, 1536 + m * 128:1536 + (m + 1) * 128], hj(k)) for k in range(KC)], [w1, hT])
                kb.op("act", lambda e, bank=bank, m=m: e.activation(out=lat[:, m, :], in_=bank[:, :], func=AF.Copy), [lat], [bank])
                kb.op("act", lambda e, bank=bank, m=m: e.activation(out=sq[:, m, :], in_=bank[:, :], func=AF.Square), [sq], [bank])
            for i in range(2):
                bank = ps[2 + i]
                kb.mm(bank, bank[:, :], [(ones_b[:], sq[:, 2 * i + c, :]) for c in range(2)], [ones_b, sq])
                kb.op("act", lambda e, bank=bank: e.activation(out=lrs[:], in_=bank[:, :], func=AF.Sqrt, bias=eps_ap, scale=1.0 / 256), [lrs], [bank, cst])
                kb.op("dve", lambda e, i=i: e.reciprocal(out=lrstd[i][:], in_=lrs[:]), [lrstd[i]], [lrs])
                for c in range(2):
                    gcol = 16 + 2 * i + c
                    kb.op("dve", lambda e, i=i, c=c, gcol=gcol: e.scalar_tensor_tensor(
                        out=latn[:, 2 * i + c, :], in0=lat[:, 2 * i + c, :], scalar=featB[:, gcol:gcol + 1], in1=lrstd[i][:],
                        op0=ALU.mult, op1=ALU.mult), [latn], [lat, featB, lrstd[i]])
            bank = ps[4]
            kb.mm(bank, bank[:, :], [(w1[:, k, 2112:2240], hj(k)) for k in range(KC)], [w1, hT])
            kb.op("act", lambda e, bank=bank: e.activation(out=kpe_raw[:], in_=bank[:, :], func=AF.Copy), [kpe_raw], [bank])
            kb.op("act", lambda e, bank=bank: e.activation(out=sq[:, 6, :], in_=bank[:, :], func=AF.Square), [sq], [bank])

            def head_norm_rope(h, nope_bank, rope_src_buf, rope_src_ap, rope_sq_ap, rope_sq_buf, gn_col, gr_col, dst, extra_scale):
                kb.op("act", lambda e: e.activation(out=sq[:, 4, :], in_=nope_bank[:, :], func=AF.Square), [sq], [nope_bank])
                bs = ps[7]
                kb.pe_begin([bs], [ones_b, sq, rope_sq_buf])
                nc.tensor.matmul(bs[:, :], lhsT=ones_b[:], rhs=sq[:, 4, :], start=True, stop=False)
                inst = nc.tensor.matmul(bs[:, :], lhsT=ones_b[0:64, :], rhs=rope_sq_ap, start=False, stop=True)
                kb.pe_end(inst, [bs], [ones_b, sq, rope_sq_buf])
                kb.op("act", lambda e: e.activation(out=hrs[:], in_=bs[:, :], func=AF.Sqrt, bias=eps_ap, scale=1.0 / QK_HD), [hrs], [bs, cst])
                kb.op("dve", lambda e: e.reciprocal(out=hrstd[:], in_=hrs[:]), [hrstd], [hrs])
                if extra_scale != 1.0:
                    kb.op("dve", lambda e: e.tensor_scalar(out=hrstd[:], in0=hrstd[:], scalar1=extra_scale, scalar2=None, op0=ALU.mult), [hrstd], [hrstd])
                kb.op("dve", lambda e: e.scalar_tensor_tensor(out=dst[:, 2 * h, :], in0=nope_bank[:, :], scalar=featB[:, gn_col:gn_col + 1],
                                                               in1=hrstd[:], op0=ALU.mult, op1=ALU.mult), [dst], [nope_bank, featB, hrstd])
                kb.op("dve", lambda e: e.scalar_tensor_tensor(out=ur[:], in0=rope_src_ap, scalar=featB[:, gr_col:gr_col + 1],
                                                               in1=hrstd[:], op0=ALU.mult, op1=ALU.mult), [ur], [rope_src_buf, featB, hrstd])
                kb.op("dve", lambda e: e.tensor_tensor(out=ur[:], in0=ur[:], in1=trig[:, cols], op=ALU.mult), [ur], [ur, trig])
                kb.op("act", lambda e: e.activation(out=urs[0:64, :], in_=ur[64:128, :], func=AF.Copy), [urs], [ur])
                kb.op("dve", lambda e: e.tensor_tensor(out=dst[0:64, 2 * h + 1, :], in0=ur[0:64, :], in1=urs[0:64, :], op=ALU.add), [dst], [ur, urs])

            for h in range(4):
                bn = ps[h % 2]
                kb.mm(bn, bn[:, :], [(wq_n[:, c, h * 128:(h + 1) * 128], latn[:, c, :]) for c in range(2)], [wq_n, latn])
                br = ps[2 + (h % 2)]
                kb.mm(br, br[:, :], [(wq_r[:, c, h * 128:(h + 1) * 128], latn[:, c, :]) for c in range(2)], [wq_r, latn])
                kb.op("act", lambda e, br=br: e.activation(out=sq[:, 5, :], in_=br[:, :], func=AF.Square), [sq], [br])
                head_norm_rope(h, bn, br, br[:, :], sq[0:64, 5, :], sq, 20, 21, QTt, SM_SCALE)
                bk = ps[4 + (h % 2)]
                kb.mm(bk, bk[:, :], [(wkv[:, c, h * 256:h * 256 + 128], latn[:, 2 + c, :]) for c in range(2)], [wkv, latn])
                head_norm_rope(h, bk, kpe_raw, kpe_raw[:], sq[0:64, 6, :], sq, 22, 23, KTt, 1.0)
            for b in range(4):
                bank = ps[b % 2]
                kb.pe_begin([bank], [latn, wkv])
                inst = None
                for h in range(4):
                    for c in range(2):
                        inst = nc.tensor.matmul(bank[:, h * 128:(h + 1) * 128], lhsT=latn[:, 2 + c, b * 128:(b + 1) * 128],
                                                rhs=wkv[:, c, h * 256 + 128:h * 256 + 256], start=(c == 0), stop=(c == 1))
                kb.pe_end(inst, [bank], [latn, wkv])
                kb.op("act", lambda e, bank=bank, b=b: e.activation(out=Vt[:, b, :], in_=bank[:, :], func=AF.Copy), [Vt], [bank])
            kb.dma_out("sp", o_QT[:, :, cols], QTt, QTt[:])
            kb.dma_out("sp", o_KT[:, :, cols], KTt, KTt[:])
            kb.dma_out("sp", o_V[:, j * 4:(j + 1) * 4, :], Vt, Vt[:])
        kb.barrier()


def build_phase2(nc, kb, es, din, dout, env):
    ps, xT, ident, identb, ones_b, cst, featA, featB, modT, scl = (env[k] for k in (
        "ps", "xT", "ident", "identb", "ones_b", "cst", "featA", "featB", "modT", "scl"))
    rms_adaln, norm_tmps, load_w, ffn_blocks, make_blocks, ffn_ring = (env[k] for k in (
        "rms_adaln", "norm_tmps", "load_w", "ffn_blocks", "make_blocks", "ffn_ring"))
    eps_ap = cst[:, 3:4]

    d_x1T = din("x1T", [P, KC, NTOK])
    d_QT = din("QT", [P, 8, NTOK], BF16)
    d_KTg = din("KTg", [32, 4, P, 2, T], BF16)
    d_Vg = din("Vg", [32, 4, P, 4, P], BF16)
    d_KTo = din("KTo", [NT, 4, P, 2, T], BF16)
    d_Vo = din("Vo", [NT, 4, P, 4, P], BF16)
    d_zx = din("zxT", [P, 4, NT, 2 + T], BF16)
    d_bg = din("bgT", [P, 4, NTOK], BF16)
    d_zone = din("zonebias", [P, NCORES])
    d_dmask = din("dmask", [P, 4, T], BF16)
    d_o_w_out = din("o_w_out", [D, D])
    d_router = din("router_w", [D, NEXP])
    d_sel = din("sel", [NEXP, NEXP, P])
    d_moe_gu = din("moe_w_gu", [NEXP, D, 2 * D_FFE])
    d_moe_dn = din("moe_w_down", [NEXP * D_FFE, D])
    o_out = dout("out", [NTOK, D])

    for k in range(KC):
        kb.dma_in("sp", xT, xT[:, k, :], d_x1T[:, k, :])

    with ExitStack() as e5:
        yT = kb.sb(e5, "yT1", [P, KC, NTOK], BF16)
        QT = kb.sb(e5, "QT", [P, 8, NTOK], BF16)
        for c in range(8):
            kb.dma_in("sp", QT, QT[:, c, :], d_QT[:, c, :])
        zone = kb.sb(e5, "zone", [P, NCORES], F32)
        dmask = kb.sb(e5, "dmask", [P, 4, T], BF16)
        kb.dma_in("sp", zone, zone[:], d_zone[:, :])
        kb.dma_in("sp", dmask, dmask[:], d_dmask[:, :, :])
        w_out = kb.sb(e5, "w_out1", [P, KC, D], BF16)
        src_out = d_o_w_out.rearrange("(k p) f -> p k f", p=P)
        for q in range(2):
            load_w("pool", w_out, lambda a, b, q=q: w_out[:, a:b, q * 512:(q + 1) * 512], src_out, KC, q * 512, (q + 1) * 512)
        with ExitStack() as e5a:
            zx = kb.sb(e5a, "zx", [P, 4, NT, 2 + T], BF16)
            bgs = kb.sb(e5a, "bgs", [P, 4, NTOK], BF16)
            cacc = [kb.sb(e5a, f"cacc{i}", [P, T], F32) for i in range(2)]
            for m in range(4):
                kb.dma_in("sp", zx, zx[:, m, :, :], d_zx[:, m, :, :])
                kb.dma_in("sp", bgs, bgs[:, m, :], d_bg[:, m, :])
            for j in range(NT):
                for m in range(4):
                    ca = cacc[m % 2]
                    kb.op("dve", lambda e, ca=ca, m=m, j=j: e.tensor_scalar(out=ca[:], in0=zx[:, m, j, 0:T], scalar1=featB[:, 4 + m:5 + m],
                                                                         scalar2=None, op0=ALU.mult), [ca], [zx, featB])
                    kb.op("dve", lambda e, ca=ca, m=m, j=j: e.scalar_tensor_tensor(out=ca[:], in0=zx[:, m, j, 1:T + 1], scalar=featB[:, 8 + m:9 + m],
                                                                                 in1=ca[:], op0=ALU.mult, op1=ALU.add), [ca], [zx, featB, ca])
                    kb.op("dve", lambda e, ca=ca, m=m, j=j: e.scalar_tensor_tensor(out=ca[:], in0=zx[:, m, j, 2:T + 2], scalar=featB[:, 12 + m:13 + m],
                                                                                 in1=ca[:], op0=ALU.mult, op1=ALU.add), [ca], [zx, featB, ca])
                    kb.op("dve", lambda e, ca=ca, m=m, j=j: e.tensor_tensor(out=yT[:, m, j * T:(j + 1) * T], in0=ca[:], in1=bgs[:, m, j * T:(j + 1) * T],
                                                                          op=ALU.mult), [yT], [ca, bgs])
            kb.barrier()

        with ExitStack() as e5b:
            NKB = 6
            NPT = 6
            kbuf = [kb.sb(e5b, f"kbuf{i}", [P, 2, T], BF16) for i in range(NKB)]
            vbuf = [kb.sb(e5b, f"vbuf{i}", [P, 4, P], BF16) for i in range(NKB)]
            pT = [kb.sb(e5b, f"pT{i}", [P, T], BF16) for i in range(NPT)]
            rinv = [kb.sb(e5b, f"rinv{i}", [P, T], F32) for i in range(2)]
            racc = [[kb.sb(e5b, f"racc{i}_{k}", [P, T], F32) for k in range(2)] for i in range(2)]
            ones_f = kb.sb(e5b, "ones_f", [P, P], F32)
            kb.op("dve", lambda e: e.memset(ones_f[:], 1.0), [ones_f], [])
            LA = 3
            items = []
            hi = 0
            tcount = 0
            for j in range(NT):
                klist = [("c", g) for g in range(8 * j)] + [("z", 8 * j + i) for i in range(NCORES)] + [("d", None)]
                for h in range(4):
                    accO, accS = ps[hi % 2], ps[2 + (hi % 2)]
                    hi += 1
                    for ti, (kind, g) in enumerate(klist):
                        for sblk in range(4):
                            items.append(dict(j=j, h=h, ti=ti, kind=kind, g=g, sblk=sblk, tile=tcount, accO=accO, accS=accS, hidx=hi - 1,
                                              st=(ti == 0 and sblk == 0), en=(ti == len(klist) - 1 and sblk == 3),
                                              lasth=(h == 3)))
                        tcount += 1
            tiles = {}
            for it in items:
                tiles.setdefault(it["tile"], it)
            loaded = set()

            def load_tile(t):
                if t in loaded or t not in tiles:
                    return
                loaded.add(t)
                it = tiles[t]
                kbf, vbf = kbuf[t % NKB], vbuf[t % NKB]
                if it["kind"] == "d":
                    kb.dma_in("sp", kbf, kbf[:], d_KTo[it["j"], it["h"]])
                    kb.dma_in("sp", vbf, vbf[:], d_Vo[it["j"], it["h"]])
                else:
                    kb.dma_in("sp", kbf, kbf[:], d_KTg[it["g"], it["h"]])
                    kb.dma_in("sp", vbf, vbf[:], d_Vg[it["g"], it["h"]])

            def emit_qk(n):
                it = items[n]
                if it["sblk"] == 0:
                    load_tile(it["tile"])
                    load_tile(it["tile"] + 1)
                    load_tile(it["tile"] + 2)
                j, h, sblk, kind, g = it["j"], it["h"], it["sblk"], it["kind"], it["g"]
                qcols = slice(j * T, (j + 1) * T)
                kbf = kbuf[it["tile"] % NKB]
                sb_ = ps[4 + (n % 4)]
                pt = pT[n % NPT]
                kb.pe_begin([sb_], [kbf, QT])
                nc.tensor.matmul(sb_[:, :], lhsT=kbf[:, 0, sblk * 128:(sblk + 1) * 128], rhs=QT[:, 2 * h, qcols], start=True, stop=False)
                inst = nc.tensor.matmul(sb_[:, :], lhsT=kbf[:, 1, sblk * 128:(sblk + 1) * 128], rhs=QT[:, 2 * h + 1, qcols],
                                        start=False, stop=True)
                kb.pe_end(inst, [sb_], [kbf, QT])
                if kind == "z":
                    bias_ap = zone[:, (g % 8):(g % 8) + 1]
                    kb.op("act", lambda e: e.activation(out=pt[:], in_=sb_[:, :], func=AF.Exp, bias=bias_ap, scale=1.0), [pt], [sb_, zone])
                else:
                    kb.op("act", lambda e: e.activation(out=pt[:], in_=sb_[:, :], func=AF.Exp), [pt], [sb_])
                if kind == "d":
                    kb.op("pool", lambda e: e.tensor_tensor(out=pt[:], in0=pt[:], in1=dmask[:, sblk, :], op=ALU.mult), [pt], [pt, dmask])

            def emit_pv(n):
                it = items[n]
                j, h, sblk = it["j"], it["h"], it["sblk"]
                qcols = slice(j * T, (j + 1) * T)
                vbf = vbuf[it["tile"] % NKB]
                pt = pT[n % NPT]
                accO, accS = it["accO"], it["accS"]
                kb.pe_begin([accO], [vbf, pt])
                inst = nc.tensor.matmul(accO[:, :], lhsT=vbf[:, sblk, :], rhs=pt[:], start=it["st"], stop=it["en"])
                kb.pe_end(inst, [accO], [vbf, pt])
                hp = it["hidx"] % 2
                eng = "dve" if (n % 2 == 0) else "pool"
                ra = racc[hp][0 if eng == "dve" else 1]
                first_for_eng = (it["ti"] == 0 and sblk < 2)
                if first_for_eng:
                    kb.op(eng, lambda e: e.tensor_copy(out=ra[:], in_=pt[:]), [ra], [pt])
                else:
                    kb.op(eng, lambda e: e.tensor_tensor(out=ra[:], in0=ra[:], in1=pt[:], op=ALU.add), [ra], [ra, pt])
                if it["en"]:
                    kb.mm(accS, accS[:, :], [(ones_f[:], racc[hp][0][:]), (ones_f[:], racc[hp][1][:])], [ones_f, racc[hp][0], racc[hp][1]])
                    rv = rinv[h % 2]
                    kb.op("dve", lambda e: e.reciprocal(out=rv[:], in_=accS[:, :]), [rv], [accS])
                    kb.op("dve", lambda e: e.tensor_tensor(out=yT[:, 4 + h, qcols], in0=accO[:, :], in1=rv[:], op=ALU.mult),
                          [yT], [accO, rv])
                    if it["lasth"]:
                        for nn in range(KC):
                            bank = ps[4 + (nn % 4)]
                            kb.mm(bank, bank[:, :], [(w_out[:, k, nn * 128:(nn + 1) * 128], yT[:, k, qcols]) for k in range(KC)], [w_out, yT])
                            gm = modT[:, 48 + 16 + nn:48 + 17 + nn]
                            kb.op("dve", lambda e, bank=bank, nn=nn, gm=gm: e.scalar_tensor_tensor(
                                out=xT[:, nn, qcols], in0=bank[:, :], scalar=gm, in1=xT[:, nn, qcols], op0=ALU.mult, op1=ALU.add),
                                [xT], [bank, modT, xT])

            N = len(items)
            for n in range(N + LA):
                if n < N:
                    emit_qk(n)
                if n - LA >= 0:
                    emit_pv(n - LA)
            kb.barrier()

    with ExitStack() as e6:
        hT = kb.sb(e6, "hT2", [P, KC, NTOK], BF16)
        gateB = kb.sb(e6, "gateB", [P, NEXP, NTOK], BF16)
        with ExitStack() as e6a:
            tmps = norm_tmps(e6a, ps[7])
            rw = kb.sb(e6a, "rw", [P, KC, NEXP], F32)
            sel = kb.sb(e6a, "sel", [NEXP, NEXP, P], F32)
            lT = kb.sb(e6a, "lT", [NEXP, T], F32)
            lg = kb.sb(e6a, "lg", [P, 4, NEXP], F32)
            gT = kb.sb(e6a, "gT", [NEXP, NTOK], F32)
            sm = kb.sb(e6a, "sm", [P, 8], F32)
            eq1 = kb.sb(e6a, "eq1", [P, NEXP], F32)
            eq2 = kb.sb(e6a, "eq2", [P, NEXP], F32)
            l2 = kb.sb(e6a, "l2", [P, NEXP], F32)
            gts = kb.sb(e6a, "gts", [P, NEXP], F32)
            kb.dma_in("sp", rw, rw[:], d_router.rearrange("(k p) e -> p k e", p=P))
            kb.dma_in("sp", sel, sel[:], d_sel[:, :, :])
            bankR = ps[6]
            for j in range(NT):
                cols = slice(j * T, (j + 1) * T)

                def h32_cb(k, tb):
                    kb.pe_begin([bankR], [tb, rw])
                    inst = nc.tensor.matmul(bankR[0:NEXP, :], lhsT=rw[:, k, :], rhs=tb[:, 0:T], start=(k == 0), stop=(k == KC - 1))
                    kb.pe_end(inst, [bankR], [tb, rw])

                rms_adaln(e6a, lambda k: (xT, xT[:, k, cols]), lambda k: (hT, hT[:, k, cols]), T, 1, 1, tmps, h32_cb)
                kb.op("act", lambda e: e.activation(out=lT[:], in_=bankR[0:NEXP, :], func=AF.Copy), [lT], [bankR])
                bankL = ps[5]
                kb.pe_begin([bankL], [lT, ident])
                inst = None
                for b in range(4):
                    inst = nc.tensor.transpose(bankL[:, b * NEXP:(b + 1) * NEXP], lT[:, b * 128:(b + 1) * 128], ident[0:NEXP, 0:NEXP])
                kb.pe_end(inst, [bankL], [lT, ident])
                kb.op("dve", lambda e: e.tensor_copy(out=lg[:].rearrange("p b e -> p (b e)"), in_=bankL[:, 0:4 * NEXP]), [lg], [bankL])
                bankG = ps[4]
                for b in range(4):
                    lgb = lg[:, b, :]
                    kb.op("dve", lambda e, lgb=lgb: e.reduce_max(out=sm[:, 0:1], in_=lgb, axis=mybir.AxisListType.X), [sm], [lg])
                    kb.op("dve", lambda e, lgb=lgb: e.tensor_scalar(out=eq1[:], in0=lgb, scalar1=sm[:, 0:1], scalar2=None, op0=ALU.is_equal), [eq1], [lg, sm])
                    kb.op("dve", lambda e, lgb=lgb: e.scalar_tensor_tensor(out=l2[:], in0=eq1[:], scalar=-1e30, in1=lgb, op0=ALU.mult, op1=ALU.add), [l2], [eq1, lg])
                    kb.op("dve", lambda e: e.reduce_max(out=sm[:, 1:2], in_=l2[:], axis=mybir.AxisListType.X), [sm], [l2])
                    kb.op("dve", lambda e: e.tensor_scalar(out=eq2[:], in0=l2[:], scalar1=sm[:, 1:2], scalar2=None, op0=ALU.is_equal), [eq2], [l2, sm])
                    kb.op("dve", lambda e: e.tensor_tensor(out=sm[:, 2:3], in0=sm[:, 1:2], in1=sm[:, 0:1], op=ALU.subtract), [sm], [sm])
                    kb.op("act", lambda e: e.activation(out=sm[:, 3:4], in_=sm[:, 2:3], func=AF.Sigmoid, scale=-1.0), [sm], [sm])
                    kb.op("act", lambda e: e.activation(out=sm[:, 4:5], in_=sm[:, 2:3], func=AF.Sigmoid, scale=1.0), [sm], [sm])
                    kb.op("dve", lambda e: e.tensor_scalar(out=gts[:], in0=eq1[:], scalar1=sm[:, 3:4], scalar2=None, op0=ALU.mult), [gts], [eq1, sm])
                    kb.op("dve", lambda e: e.scalar_tensor_tensor(out=gts[:], in0=eq2[:], scalar=sm[:, 4:5], in1=gts[:], op0=ALU.mult, op1=ALU.add), [gts], [eq2, sm, gts])
                    kb.pe_begin([bankG], [gts, ident])
                    inst = nc.tensor.transpose(bankG[0:NEXP, b * 128:(b + 1) * 128], gts[:], ident[:])
                    kb.pe_end(inst, [bankG], [gts, ident])
                kb.op("act", lambda e, cols=cols: e.activation(out=gT[:, cols], in_=bankG[0:NEXP, :], func=AF.Copy), [gT], [bankG])
                for ex in range(NEXP):
                    bb = ps[ex % 4]
                    kb.mm(bb, bb[:, :], [(sel[:, ex, :], gT[:, cols])], [sel, gT])
                    kb.op("act", lambda e, bb=bb, ex=ex, cols=cols: e.activation(out=gateB[:, ex, cols], in_=bb[:, :], func=AF.Copy), [gateB], [bb])
            kb.barrier()
        ring = ffn_ring(e6)
        blocks = []
        for ex in range(NEXP):
            w_gu3 = d_moe_gu[ex].rearrange("(k p) f -> p k f", p=P)
            blocks += make_blocks(w_gu3, d_moe_dn, D_FFE, 0, D_FFE, ex * D_FFE,
                                  lambda j, ex=ex: (gateB, gateB[:, ex, j * T:(j + 1) * T]))
        ffn_blocks(hT, blocks, 48 + 40, ring)
        kb.barrier()

    with ExitStack() as e7:
        ost = [kb.sb(e7, f"ost{i}", [P, D], F32) for i in range(3)]
        for blk in range(16):
            o = ost[blk % 3]
            for half in range(2):
                bank = ps[(blk * 2 + half) % 4]
                kb.pe_begin([bank], [xT, ident])
                inst = None
                for kk in range(4):
                    k = half * 4 + kk
                    inst = nc.tensor.transpose(bank[:, kk * 128:(kk + 1) * 128], xT[:, k, blk * 128:(blk + 1) * 128], ident[:])
                kb.pe_end(inst, [bank], [xT, ident])
                if half == 0:
                    kb.op("act", lambda e, o=o, bank=bank: e.activation(out=o[:, 0:512], in_=bank[:, :], func=AF.Copy), [o], [bank])
                else:
                    kb.op("dve", lambda e, o=o, bank=bank: e.tensor_copy(out=o[:, 512:1024], in_=bank[:, :]), [o], [bank])
            kb.dma_out("sp", o_out[blk * 128:(blk + 1) * 128, :], o, o[:])


_PROGS = {}


def _get_prog(phase):
    if phase not in _PROGS:
        _PROGS[phase] = build_program(phase)
    return _PROGS[phase]


def _sw(r):
    return r + 32 if r < 32 else r - 32


def _common_inputs(inp):
    f = np.float32
    ident = np.eye(P, dtype=f)
    cst = np.zeros((P, 16), f)
    inv_freq = (10000.0 ** (-np.arange(0, 64, 2, dtype=np.float32) / np.float32(64))).astype(f)
    for p in range(P):
        cst[p, 0] = inv_freq[p % 32]
        cst[p, 1] = 1.0 if p >= 96 else -1.0
        cst[p, 2] = np.pi / 2 if p < 64 else 0.0
    cst[:, 3] = EPS
    vecA = np.zeros((P, P), f)
    for l in range(2):
        vecA[l * 64 + 0:l * 64 + 8] = inp["norm_mix_w"][l].reshape(8, P)
        vecA[l * 64 + 8:l * 64 + 16] = inp["norm_ffn_w"][l].reshape(8, P)
        vecA[l * 64 + 16:l * 64 + 64] = inp["ada_b"][l].reshape(48, P)
    vecB = np.zeros((32, P), f)
    vecB[0:4] = inp["b_scale"][0].reshape(4, P)
    cw = inp["c_conv_w"][0]
    for k in range(3):
        vecB[4 + k * 4:8 + k * 4] = cw[k].reshape(4, P)
    vecB[16:18] = inp["q_a_norm"][0].reshape(2, P)
    vecB[18:20] = inp["kv_norm"][0].reshape(2, P)
    idx = np.array([128 + r for r in range(64)] + [128 + _sw(r) for r in range(64)])
    qn = inp["q_norm_w"][0]
    kn = inp["k_norm_w"][0]
    vecB[20] = qn[:128]
    vecB[21] = qn[idx]
    vecB[22] = kn[:128]
    vecB[23] = kn[idx]
    cT = np.ascontiguousarray(inp["c"][0].reshape(8, P).T)
    return {"ident": ident, "cst": cst, "vecA": vecA, "vecB": vecB, "cT": cT,
            "ada_w": np.ascontiguousarray(inp["ada_w"])}


def kernel(**inputs):
    import ml_dtypes
    inp = {k: np.asarray(v) for k, v in inputs.items()}
    f = np.float32
    x = inp["x"][0]
    pos = inp["positions"][0].astype(np.int32)
    common = _common_inputs(inp)

    ridx = np.array([r for r in range(64)] + [_sw(r) for r in range(64)])
    o_w_in = inp["o_w_in"][0]
    w_kpe = np.ascontiguousarray(o_w_in[:, 2048 + ridx])
    w_uq = inp["w_uq"][0]
    w_uq_n = np.ascontiguousarray(np.concatenate([w_uq[:, h * 192:h * 192 + 128] for h in range(4)], axis=1))
    w_uq_r = np.ascontiguousarray(np.concatenate([w_uq[:, h * 192 + 128 + ridx] for h in range(4)], axis=1))
    a_w_s = inp["a_w_s"][0]
    wsT = np.ascontiguousarray(np.transpose(a_w_s, (0, 2, 1)))
    trimask = np.triu(np.ones((P, P), f))
    lnwb = np.ascontiguousarray(np.broadcast_to(np.stack([inp["a_ln_w"][0], inp["a_ln_b"][0]])[None], (P, 2, 512)))
    bs = inp["a_b_s"][0]
    bsb = np.ascontiguousarray(np.broadcast_to(np.tile(bs, (1, 4))[None], (P, 4, 512)))
    shared1 = dict(common)
    shared1.update({
        "e_w_in": inp["e_w_in"][0], "e_w_out": inp["e_w_out"][0], "lnwb": lnwb, "bsb": bsb, "wsT": wsT, "trimask": trimask,
        "b_w_grp": inp["b_w_grp"][0], "ffn_w_gu": inp["ffn_w_gu"][0], "ffn_w_down": inp["ffn_w_down"][0],
        "o_w_in": o_w_in, "w_kpe": w_kpe, "w_uq_n": w_uq_n, "w_uq_r": w_uq_r, "w_ukv": inp["w_ukv"][0],
    })
    in_maps = []
    for c in range(NCORES):
        gs = [8 * j + c for j in range(NT)]
        xc = np.concatenate([x[512 * g:512 * (g + 1)] for g in gs], axis=0)
        xh = np.zeros((NT * HALO, D), f)
        hflag = np.ones((P, NT), f)
        for j, g in enumerate(gs):
            if g == 0:
                hflag[:, j] = 0.0
            else:
                xh[j * HALO:(j + 1) * HALO] = x[512 * g - HALO:512 * g]
        corr = np.ones((P, 4, HALO), f)
        if c == 0:
            for m, win in enumerate(POOL_WINDOWS):
                for t in range(HALO):
                    corr[:, m, t] = win / min(t + 1, win)
        posc = np.concatenate([pos[512 * g:512 * (g + 1)] for g in gs])
        posb = np.ascontiguousarray(np.broadcast_to(posc[None, :], (P, NTOK))).astype(np.int32)
        m = dict(shared1)
        m.update({"x": np.ascontiguousarray(xc), "xh": xh, "hflag": hflag, "corr": corr, "posb": posb})
        in_maps.append(m)
    nc1 = _get_prog(1)
    res1 = run_bass_kernel_spmd(nc1, in_maps, core_ids=list(range(NCORES))).results

    bf = ml_dtypes.bfloat16
    KTg = np.zeros((32, 4, P, 2, T), bf)
    Vg = np.zeros((32, 4, P, 4, P), bf)
    ztail = np.zeros((32, P, 4, 2), bf)
    for c in range(NCORES):
        KT = np.asarray(res1[c]["KT"]).reshape(P, 4, 2, NT, T)
        V = np.asarray(res1[c]["V"]).reshape(P, NT, 4, 4, P)
        z = np.asarray(res1[c]["zT"]).reshape(P, 4, NT, T)
        for j in range(NT):
            g = 8 * j + c
            KTg[g] = np.transpose(KT[:, :, :, j, :], (1, 0, 2, 3))
            Vg[g] = np.transpose(V[:, j], (2, 0, 1, 3))
            ztail[g] = z[:, :, j, T - 2:]
    dmask = np.zeros((P, 4, T), f)
    for s in range(4):
        kp = np.arange(P)[:, None] + 128 * s
        dmask[:, s, :] = (kp <= np.arange(T)[None, :]).astype(f)
    dmask = dmask.astype(bf)
    sel = np.zeros((NEXP, NEXP, P), f)
    for e in range(NEXP):
        sel[e, e, :] = 1.0
    shared2 = dict(common)
    shared2.update({
        "KTg": KTg, "Vg": Vg, "dmask": dmask, "sel": sel, "o_w_out": inp["o_w_out"][0], "router_w": inp["router_w"][0],
        "moe_w_gu": inp["moe_w_gu"][0], "moe_w_down": np.ascontiguousarray(inp["moe_w_down"][0].reshape(NEXP * D_FFE, D)),
    })
    in_maps2 = []
    for c in range(NCORES):
        z = np.asarray(res1[c]["zT"]).reshape(P, 4, NT, T)
        zx = np.zeros((P, 4, NT, 2 + T), bf)
        zx[:, :, :, 2:] = z
        for j in range(NT):
            g = 8 * j + c
            if g > 0:
                zx[:, :, j, 0:2] = ztail[g - 1]
        zone = np.zeros((P, NCORES), f)
        zone[:, c:] = NEG
        m = dict(shared2)
        m.update({"x1T": np.asarray(res1[c]["x1T"]), "QT": np.asarray(res1[c]["QT"]),
                  "KTo": np.ascontiguousarray(KTg[[8 * j + c for j in range(NT)]]),
                  "Vo": np.ascontiguousarray(Vg[[8 * j + c for j in range(NT)]]),
                  "zxT": zx, "bgT": np.asarray(res1[c]["bgT"]), "zonebias": zone})
        in_maps2.append(m)
    nc2 = _get_prog(2)
    res2 = run_bass_kernel_spmd(nc2, in_maps2, core_ids=list(range(NCORES))).results
    out = np.zeros((SEQ, D), f)
    for c in range(NCORES):
        oc = np.asarray(res2[c]["out"])
        for j in range(NT):
            g = 8 * j + c
            out[512 * g:512 * (g + 1)] = oc[j * T:(j + 1) * T]
    return out[None]
```
